# Optimizing a Trainium2 kernel written in Bass

```python
import math
import jax
import jax.numpy as jnp
from jax import lax
import numpy as np


D_MODEL = 1024
BATCH = 1
SEQ = 16384
DEPTH = 4

N_MIXERS = 3
N_LAYERS_A = (DEPTH + 2) // 3
N_LAYERS_B = (DEPTH + 1) // 3
N_LAYERS_C = DEPTH // 3
DEEPNORM_ALPHA = (2.0 * DEPTH) ** 0.25
DEEPNORM_BETA = (8.0 * DEPTH) ** -0.25
LN_EPS = 1e-5

RW_HEAD = 64
RW_HEADS = D_MODEL // RW_HEAD
RW_DECAY_LORA = 64
RW_AAA_LORA = 64
RW_GATE_LORA = 160
RW_GN_EPS = 64e-5

MB_DI = 2 * D_MODEL
MB_HEADDIM = 64
MB_HEADS = MB_DI // MB_HEADDIM
MB_GROUPS = 4
MB_HPG = MB_HEADS // MB_GROUPS
MB_STATE = 128
MB_CONV = 4
MB_CHUNK = 128
MB_CONV_DIM = MB_DI + 2 * MB_GROUPS * MB_STATE
MB_PROJ = MB_DI + MB_CONV_DIM + MB_HEADS
MB_NORM_EPS = 1e-5

GL_HEADS = 4
GL_KD = D_MODEL // 2
GL_VD = D_MODEL
GL_DK = GL_KD // GL_HEADS
GL_DV = GL_VD // GL_HEADS
GL_GATE_LORA = 16
GL_GATE_NORM = 16.0
GL_CHUNK = 64
GL_PROJ = 2 * GL_KD + 2 * GL_VD + GL_GATE_LORA
GL_NORM_EPS = 1e-5

MOE_GROUPS = 4
MOE_PER_GROUP = 8
MOE_EXPERTS = MOE_GROUPS * MOE_PER_GROUP
MOE_TOPK = 2
MOE_FF = 512
MOE_BLOCK = 128

kernel_name = 'hybrid_rwkv7_mamba2_gla_hmoe_deepnorm'


def layer_norm(x, g, b):
    xf = x.astype(jnp.float32)
    mu = jnp.mean(xf, -1, keepdims=True)
    var = jnp.mean(jnp.square(xf - mu), -1, keepdims=True)
    return ((xf - mu) * lax.rsqrt(var + LN_EPS) * g + b).astype(x.dtype)


def token_shift(x):
    return jnp.pad(x, ((0, 0), (1, 0), (0, 0)))[:, :-1]


def causal_depthwise_conv(x, w, b):
    width, ch = w.shape
    y = lax.conv_general_dilated(x, w[:, None, :].astype(x.dtype), window_strides=(1,),
                                 padding=[(width - 1, 0)],
                                 dimension_numbers=('NWC', 'WIO', 'NWC'),
                                 feature_group_count=ch)
    return y + b


def rwkv7_mix(x, mu, w_rkv, w0, w1, w2, a0, a1, a2, g1, g2, k_k, k_a, r_k, lnx_g, lnx_b, w_o):
    f32 = jnp.float32
    bsz, seq, dm = x.shape
    H, N = RW_HEADS, RW_HEAD
    xx = token_shift(x) - x
    xs = x[None] + xx[None] * mu[:, None, None, :]
    r, k, v = jnp.einsum('pbsd,pde->pbse', xs[:3], w_rkv)
    xw, xa, xg = xs[3], xs[4], xs[5]
    w_log = -jax.nn.softplus(-(w0 + jnp.tanh(xw @ w1) @ w2)) - 0.5
    decay = jnp.exp(-jnp.exp(w_log.astype(f32)))
    a = jax.nn.sigmoid((a0 + (xa @ a1) @ a2).astype(f32))
    g = jax.nn.sigmoid(xg @ g1) @ g2

    def heads(t):
        return t.astype(f32).reshape(bsz, seq, H, N)

    kk = heads(k * k_k)
    kk = kk / jnp.maximum(jnp.sqrt(jnp.sum(kk * kk, -1, keepdims=True)), 1e-12)
    k_h = heads(k.astype(f32) * (1.0 + (a - 1.0) * k_a.astype(f32)))
    r_h, v_h, a_h, w_h = heads(r), heads(v), heads(a), heads(decay)

    def step(state, inp):
        r_t, w_t, k_t, v_t, kk_t, a_t = inp
        sa = jnp.einsum('bhvk,bhk->bhv', state, -kk_t)
        state = (state * w_t[:, :, None, :] + sa[..., None] * (kk_t * a_t)[:, :, None, :]
                 + v_t[..., None] * k_t[:, :, None, :])
        return state, jnp.einsum('bhvk,bhk->bhv', state, r_t)

    seq_in = tuple(jnp.moveaxis(t, 1, 0) for t in (r_h, w_h, k_h, v_h, kk, a_h))
    _, y = lax.scan(step, jnp.zeros((bsz, H, N, N), f32), seq_in)
    y = jnp.moveaxis(y, 0, 1)
    mu_y = jnp.mean(y, -1, keepdims=True)
    var_y = jnp.mean(jnp.square(y - mu_y), -1, keepdims=True)
    y = ((y - mu_y) * lax.rsqrt(var_y + RW_GN_EPS)).reshape(bsz, seq, dm) * lnx_g + lnx_b
    bonus = jnp.sum(r_h * k_h * r_k.astype(f32), -1, keepdims=True) * v_h
    y = (y + bonus.reshape(bsz, seq, dm)) * g
    return (y.astype(x.dtype) @ w_o).astype(x.dtype)


def mamba2_mix(x, w_in, conv_w, conv_b, dt_bias, a_log, d_skip, norm_g, w_out):
    f32 = jnp.float32
    bsz, seq, _ = x.shape
    G, E, P, N, L = MB_GROUPS, MB_HPG, MB_HEADDIM, MB_STATE, MB_CHUNK
    nc = seq // L
    zxbcdt = x @ w_in
    z, xbc, dt = jnp.split(zxbcdt, [MB_DI, MB_DI + MB_CONV_DIM], axis=-1)
    xbc = jax.nn.silu(causal_depthwise_conv(xbc, conv_w, conv_b))
    xs, bm, cm = jnp.split(xbc, [MB_DI, MB_DI + G * N], axis=-1)
    dt = jax.nn.softplus((dt + dt_bias).astype(f32))
    a = -jnp.exp(a_log.astype(f32)).reshape(G, E)

    xs_c = xs.astype(f32).reshape(bsz, nc, L, G, E, P)
    b_c = bm.astype(f32).reshape(bsz, nc, L, G, N)
    c_c = cm.astype(f32).reshape(bsz, nc, L, G, N)
    dt_c = dt.reshape(bsz, nc, L, G, E)
    da_cs = jnp.cumsum(dt_c * a, axis=2)
    xdt = xs_c * dt_c[..., None]

    causal = jnp.tril(jnp.ones((L, L), bool))[:, :, None, None]
    seg = da_cs[:, :, :, None] - da_cs[:, :, None, :]
    decay_ls = jnp.exp(jnp.where(causal, seg, -jnp.inf))
    scores = jnp.einsum('bclgn,bcsgn->bclsg', c_c, b_c)
    y_diag = jnp.einsum('bclsg,bclsge,bcsgep->bclgep', scores, decay_ls, xdt)

    decay_to_end = jnp.exp(da_cs[:, :, -1:] - da_cs)
    chunk_states = jnp.einsum('bclgn,bclge,bclgep->bcgepn', b_c, decay_to_end, xdt)
    chunk_decay = jnp.exp(da_cs[:, :, -1])

    def carry(h, inp):
        s_c, dec_c = inp
        return h * dec_c[..., None, None] + s_c, h

    _, prev = lax.scan(carry, jnp.zeros((bsz, G, E, P, N), f32),
                       (jnp.moveaxis(chunk_states, 1, 0), jnp.moveaxis(chunk_decay, 1, 0)))
    prev = jnp.moveaxis(prev, 0, 1)
    y_off = jnp.einsum('bclgn,bcgepn,bclge->bclgep', c_c, prev, jnp.exp(da_cs))
    y = y_diag + y_off + xs_c * d_skip.astype(f32).reshape(G, E)[:, :, None]
    y = y.reshape(bsz, seq, MB_DI) * jax.nn.silu(z.astype(f32))
    yg = y.reshape(bsz, seq, G, MB_DI // G)
    yg = yg * lax.rsqrt(jnp.mean(yg * yg, -1, keepdims=True) + MB_NORM_EPS)
    y = yg.reshape(bsz, seq, MB_DI) * norm_g
    return (y.astype(x.dtype) @ w_out).astype(x.dtype)


def gla_mix(x, w_in, gk_w2, gk_b, norm_g, w_out):
    f32 = jnp.float32
    bsz, seq, _ = x.shape
    H, DK, DV, C = GL_HEADS, GL_DK, GL_DV, GL_CHUNK
    n = seq // C
    proj = x @ w_in
    q, k, v, g, gk_lr = jnp.split(proj, [GL_KD, 2 * GL_KD, 2 * GL_KD + GL_VD, 2 * GL_KD + 2 * GL_VD], axis=-1)
    log_alpha = jax.nn.log_sigmoid((gk_lr @ gk_w2 + gk_b).astype(f32)) / GL_GATE_NORM
    q = q.astype(f32).reshape(bsz, n, C, H, DK) * (DK ** -0.5)
    k = k.astype(f32).reshape(bsz, n, C, H, DK)
    v = v.astype(f32).reshape(bsz, n, C, H, DV)
    b_cs = jnp.cumsum(log_alpha.reshape(bsz, n, C, H, DK), axis=2)
    q_in = q * jnp.exp(b_cs)
    k_in = k * jnp.exp(-b_cs)
    k_end = k * jnp.exp(b_cs[:, :, -1:] - b_cs)
    mask = jnp.tril(jnp.ones((C, C), bool))
    attn = jnp.where(mask, jnp.einsum('bnihk,bnjhk->bnhij', q_in, k_in), 0.0)
    o_intra = jnp.einsum('bnhij,bnjhv->bnihv', attn, v)
    chunk_kv = jnp.einsum('bnjhk,bnjhv->bnhkv', k_end, v)
    chunk_decay = jnp.exp(b_cs[:, :, -1])

    def carry(s, inp):
        kv_c, dec_c = inp
        return s * dec_c[..., None] + kv_c, s

    _, prev = lax.scan(carry, jnp.zeros((bsz, H, DK, DV), f32),
                       (jnp.moveaxis(chunk_kv, 1, 0), jnp.moveaxis(chunk_decay, 1, 0)))
    prev = jnp.moveaxis(prev, 0, 1)
    o = o_intra + jnp.einsum('bnihk,bnhkv->bnihv', q_in, prev)
    o = o.reshape(bsz, seq, H, DV)
    o = o * lax.rsqrt(jnp.mean(o * o, -1, keepdims=True) + GL_NORM_EPS) * norm_g
    o = o.reshape(bsz, seq, GL_VD) * jax.nn.silu(g.astype(f32))
    return (o.astype(x.dtype) @ w_out).astype(x.dtype)


def hier_moe(x, w_group, b_group, w_route, b_route, w_in, w_down):
    f32 = jnp.float32
    bsz, seq, dm = x.shape
    T = bsz * seq
    E, EPG, K, BLK = MOE_EXPERTS, MOE_PER_GROUP, MOE_TOPK, MOE_BLOCK
    xt = x.reshape(T, dm)
    group_probs = jax.nn.softmax((xt @ w_group + b_group).astype(f32), -1)
    p_group, g_idx = lax.top_k(group_probs, 1)
    exp_logits = (xt @ w_route + b_route).astype(f32).reshape(T, MOE_GROUPS, EPG)
    sel_logits = jnp.take_along_axis(exp_logits, g_idx[:, :, None], axis=1)[:, 0]
    p_exp, e_local = lax.top_k(jax.nn.softmax(sel_logits, -1), K)
    gates = p_group * p_exp / jnp.sum(p_exp, -1, keepdims=True)
    expert_id = g_idx * EPG + e_local

    A = T * K
    flat_e = expert_id.reshape(A)
    flat_tok = jnp.repeat(jnp.arange(T, dtype=jnp.int32), K)
    flat_gate = gates.reshape(A)
    order = jnp.argsort(flat_e)
    se = flat_e[order]
    tok_sorted = flat_tok[order]
    counts = jnp.bincount(flat_e, length=E)
    starts = jnp.cumsum(counts) - counts
    padded = (counts + BLK - 1) // BLK * BLK
    pad_end = jnp.cumsum(padded)
    pad_start = pad_end - padded
    dest = pad_start[se] + (jnp.arange(A) - starts[se])
    n_blocks = (A + E * (BLK - 1) + BLK - 1) // BLK
    buf = jnp.zeros((n_blocks * BLK, dm), x.dtype).at[dest].set(xt[tok_sorted])
    block_expert = jnp.minimum(jnp.searchsorted(pad_end, jnp.arange(n_blocks) * BLK, side='right'), E - 1)

    def expert_block(blk):
        xb, e = blk
        hg, hu = jnp.split(xb @ w_in[e], 2, axis=-1)
        return (jax.nn.silu(hg) * hu) @ w_down[e]

    y_buf = lax.map(expert_block, (buf.reshape(n_blocks, BLK, dm), block_expert)).reshape(n_blocks * BLK, dm)
    y_assign = (y_buf[dest] * flat_gate[order][:, None]).astype(x.dtype)
    y = jnp.zeros((T, dm), x.dtype).at[tok_sorted].add(y_assign)
    return y.reshape(bsz, seq, dm)


def setup_inputs(seed: int = 0) -> dict:
    key = jax.random.key(seed)
    ks = iter(jax.random.split(key, 48))
    f32 = jnp.float32
    D = D_MODEL
    NA, NB, NC = N_LAYERS_A, N_LAYERS_B, N_LAYERS_C

    def nrm(shape, scale):
        return jax.random.normal(next(ks), shape, f32) * scale

    def unif(shape, lo, hi):
        return jax.random.uniform(next(ks), shape, f32, lo, hi)

    inp = {}
    inp['x'] = nrm((BATCH, SEQ, D), 1.0)
    inp['ln_g'] = 1.0 + nrm((DEPTH, 2, D), 0.01)
    inp['ln_b'] = nrm((DEPTH, 2, D), 0.01)
    inp['rw_mu'] = unif((NA, 6, D), 0.0, 1.0)
    inp['rw_w_rkv'] = nrm((NA, 3, D, D), D ** -0.5)
    inp['rw_w0'] = unif((NA, D), -6.0, -1.0)
    inp['rw_w1'] = nrm((NA, D, RW_DECAY_LORA), D ** -0.5)
    inp['rw_w2'] = nrm((NA, RW_DECAY_LORA, D), 0.1 * RW_DECAY_LORA ** -0.5)
    inp['rw_a0'] = nrm((NA, D), 0.1)
    inp['rw_a1'] = nrm((NA, D, RW_AAA_LORA), D ** -0.5)
    inp['rw_a2'] = nrm((NA, RW_AAA_LORA, D), 0.1 * RW_AAA_LORA ** -0.5)
    inp['rw_g1'] = nrm((NA, D, RW_GATE_LORA), D ** -0.5)
    inp['rw_g2'] = nrm((NA, RW_GATE_LORA, D), RW_GATE_LORA ** -0.5)
    inp['rw_k_k'] = 0.85 + nrm((NA, D), 0.01)
    inp['rw_k_a'] = 1.0 + nrm((NA, D), 0.01)
    inp['rw_r_k'] = nrm((NA, RW_HEADS, RW_HEAD), 0.1)
    inp['rw_lnx_g'] = 1.0 + nrm((NA, D), 0.01)
    inp['rw_lnx_b'] = nrm((NA, D), 0.01)
    inp['rw_w_o'] = nrm((NA, D, D), D ** -0.5 * DEEPNORM_BETA)
    inp['mb_w_in'] = nrm((NB, D, MB_PROJ), D ** -0.5)
    inp['mb_conv_w'] = nrm((NB, MB_CONV, MB_CONV_DIM), MB_CONV ** -0.5)
    inp['mb_conv_b'] = nrm((NB, MB_CONV_DIM), 0.01)
    dt0 = jnp.exp(unif((NB, MB_HEADS), math.log(1e-3), math.log(1e-1)))
    inp['mb_dt_bias'] = dt0 + jnp.log(-jnp.expm1(-dt0))
    inp['mb_a_log'] = jnp.log(unif((NB, MB_HEADS), 1.0, 16.0))
    inp['mb_d'] = 1.0 + nrm((NB, MB_HEADS), 0.01)
    inp['mb_norm_g'] = 1.0 + nrm((NB, MB_DI), 0.01)
    inp['mb_w_out'] = nrm((NB, MB_DI, D), MB_DI ** -0.5 * DEEPNORM_BETA)
    inp['gl_w_in'] = nrm((NC, D, GL_PROJ), D ** -0.5)
    inp['gl_gk_w2'] = nrm((NC, GL_GATE_LORA, GL_KD), GL_GATE_LORA ** -0.5)
    inp['gl_gk_b'] = nrm((NC, GL_KD), 0.1)
    inp['gl_norm_g'] = 1.0 + nrm((NC, GL_DV), 0.01)
    inp['gl_w_out'] = nrm((NC, GL_VD, D), GL_VD ** -0.5 * DEEPNORM_BETA)
    inp['moe_w_group'] = nrm((DEPTH, D, MOE_GROUPS), D ** -0.5)
    inp['moe_b_group'] = nrm((DEPTH, MOE_GROUPS), 0.01)
    inp['moe_w_route'] = nrm((DEPTH, D, MOE_EXPERTS), D ** -0.5)
    inp['moe_b_route'] = nrm((DEPTH, MOE_EXPERTS), 0.01)
    inp['moe_w_in'] = nrm((DEPTH, MOE_EXPERTS, D, 2 * MOE_FF), D ** -0.5)
    inp['moe_w_down'] = nrm((DEPTH, MOE_EXPERTS, MOE_FF, D), MOE_FF ** -0.5 * DEEPNORM_BETA)
    return inp


def reference(x, ln_g, ln_b, rw_mu, rw_w_rkv, rw_w0, rw_w1, rw_w2, rw_a0, rw_a1, rw_a2,
              rw_g1, rw_g2, rw_k_k, rw_k_a, rw_r_k, rw_lnx_g, rw_lnx_b, rw_w_o,
              mb_w_in, mb_conv_w, mb_conv_b, mb_dt_bias, mb_a_log, mb_d, mb_norm_g, mb_w_out,
              gl_w_in, gl_gk_w2, gl_gk_b, gl_norm_g, gl_w_out,
              moe_w_group, moe_b_group, moe_w_route, moe_b_route, moe_w_in, moe_w_down):
    h = x
    for i in range(DEPTH):
        kind, j = i % N_MIXERS, i // N_MIXERS
        if kind == 0:
            m = rwkv7_mix(h, rw_mu[j], rw_w_rkv[j], rw_w0[j], rw_w1[j], rw_w2[j], rw_a0[j], rw_a1[j],
                          rw_a2[j], rw_g1[j], rw_g2[j], rw_k_k[j], rw_k_a[j], rw_r_k[j],
                          rw_lnx_g[j], rw_lnx_b[j], rw_w_o[j])
        elif kind == 1:
            m = mamba2_mix(h, mb_w_in[j], mb_conv_w[j], mb_conv_b[j], mb_dt_bias[j], mb_a_log[j],
                           mb_d[j], mb_norm_g[j], mb_w_out[j])
        else:
            m = gla_mix(h, gl_w_in[j], gl_gk_w2[j], gl_gk_b[j], gl_norm_g[j], gl_w_out[j])
        h = layer_norm(DEEPNORM_ALPHA * h + m, ln_g[i, 0], ln_b[i, 0])
        f = hier_moe(h, moe_w_group[i], moe_b_group[i], moe_w_route[i], moe_b_route[i],
                     moe_w_in[i], moe_w_down[i])
        h = layer_norm(DEEPNORM_ALPHA * h + f, ln_g[i, 1], ln_b[i, 1])
    return h
```

```python
import numpy as np
from contextlib import ExitStack
import concourse.bass as bass
import concourse.mybir as mybir
from concourse.bass_utils import run_bass_kernel_spmd

F32 = mybir.dt.float32
I32 = mybir.dt.int32
AF = mybir.ActivationFunctionType
ALU = mybir.AluOpType
AX = mybir.AxisListType

NCORES = 8
D = 1024
SEQ = 16384
DEPTH = 4
ALPHA = (2.0 * DEPTH) ** 0.25


class Prog:
    EPOCH = 30000
    DMA_K = 6

    def __init__(self, nc):
        self.nc = nc
        self.names = ['pe', 'act', 'dve', 'pool', 'sp']
        self.lists = {k: [] for k in self.names}
        self.count = {k: 0 for k in self.names}
        self.dma_count = {k: 0 for k in self.names}
        self.last_w = {}
        self.readers = {}
        self.waited = {k: {} for k in self.names}
        self.semkeys = []

    def _semkey(self, key):
        if key not in self.semkeys:
            self.semkeys.append(key)
        return key

    def _deps(self, reads, writes):
        deps = []
        for r in reads:
            if r in self.last_w:
                deps.append(self.last_w[r])
        for w in writes:
            if w in self.last_w:
                deps.append(self.last_w[w])
            deps.extend(self.readers.get(w, []))
        return deps

    def _record(self, eng, deps, fn, tok, inc, reads, writes, extra_waits=()):
        need = {}
        for (k, v) in list(deps) + list(extra_waits):
            if need.get(k, 0) < v:
                need[k] = v
        wd = self.waited[eng]
        waits = []
        for k, v in need.items():
            if wd.get(k, 0) < v:
                waits.append((k, v))
                wd[k] = v
        self.lists[eng].append((waits, fn, tok, inc))
        for r in reads:
            self.readers.setdefault(r, []).append(tok)
        for w in writes:
            self.last_w[w] = tok
            self.readers[w] = []

    def op(self, eng, fn, reads=(), writes=()):
        writes = list(writes) + [r for r in reads if r.startswith('ps') and r not in writes]
        deps = self._deps(reads, writes)
        i = self.count[eng]
        self.count[eng] = i + 1
        key = self._semkey((eng, 'c', i // self.EPOCH))
        tok = (key, (i % self.EPOCH) + 1)
        self._record(eng, deps, fn, tok, 1, reads, writes)

    def dma(self, eng, fn, reads=(), writes=()):
        deps = self._deps(reads, writes)
        i = self.dma_count[eng]
        self.dma_count[eng] = i + 1
        key = self._semkey((eng, 'd', i % self.DMA_K))
        val = 16 * (i // self.DMA_K + 1)
        extra = []
        if i >= self.DMA_K:
            extra.append((key, val - 16))
        self._record(eng, deps, fn, (key, val), 16, reads, writes, extra_waits=extra)

    def emit(self):
        nc = self.nc
        with ExitStack() as st:
            sems = {}
            for k in self.semkeys:
                sems[k] = st.enter_context(nc.semaphore("s_%s_%s_%d" % k))
            block = st.enter_context(nc.Block())
            engs = {'pe': block.tensor, 'act': block.scalar, 'dve': block.vector,
                    'pool': block.gpsimd, 'sp': block.sync}
            finals = {}
            for name in self.names:
                for (waits, fn, tok, inc) in self.lists[name]:
                    finals[tok[0]] = max(finals.get(tok[0], 0), tok[1])

            def make(name):
                lst = self.lists[name]

                def body(e):
                    for (waits, fn, tok, inc) in lst:
                        for (k, v) in waits:
                            e.wait_ge(sems[k], v)
                        fn(e).then_inc(sems[tok[0]], inc)
                    if name == 'sp':
                        for k, v in finals.items():
                            e.wait_ge(sems[k], v)
                return body
            for name in self.names:
                if self.lists[name] or name == 'sp':
                    engs[name](make(name))

    def MM(self, out, lhsT, rhs, start, stop, reads, writes):
        self.op('pe', lambda e: e.matmul(out, lhsT, rhs, start=start, stop=stop), reads, writes)

    def TR(self, out, in_, ident, reads, writes):
        self.op('pe', lambda e: e.transpose(out, in_, ident), list(reads) + ['ident'], writes)

    def ACT(self, out, in_, func, reads, writes, bias=None, scale=1.0):
        if bias is None:
            self.op('act', lambda e: e.activation(out=out, in_=in_, func=func, scale=scale), reads, writes)
        else:
            self.op('act', lambda e: e.activation(out=out, in_=in_, func=func, bias=bias, scale=scale), reads, writes)

    def TS(self, out, in0, s1, s2, op0, op1, reads, writes, eng='dve'):
        if op1 is None:
            self.op(eng, lambda e: e.tensor_scalar(out=out, in0=in0, scalar1=s1, scalar2=None, op0=op0), reads, writes)
        else:
            self.op(eng, lambda e: e.tensor_scalar(out=out, in0=in0, scalar1=s1, scalar2=s2, op0=op0, op1=op1), reads, writes)

    def TT(self, out, in0, in1, op, reads, writes, eng='dve'):
        self.op(eng, lambda e: e.tensor_tensor(out=out, in0=in0, in1=in1, op=op), reads, writes)

    def STT(self, out, in0, scalar, in1, op0, op1, reads, writes, eng='dve'):
        self.op(eng, lambda e: e.scalar_tensor_tensor(out=out, in0=in0, scalar=scalar, in1=in1, op0=op0, op1=op1), reads, writes)

    def CP(self, out, in_, reads, writes, eng='dve'):
        if eng == 'act':
            self.op('act', lambda e: e.copy(out=out, in_=in_), reads, writes)
        else:
            self.op(eng, lambda e: e.tensor_copy(out=out, in_=in_), reads, writes)

    def RED(self, out, in_, op, reads, writes):
        self.op('dve', lambda e: e.tensor_reduce(out=out, in_=in_, axis=AX.X, op=op), reads, writes)

    def RCP(self, out, in_, reads, writes):
        self.op('dve', lambda e: e.reciprocal(out=out, in_=in_), reads, writes)

    def LD(self, out, in_, writes, reads=()):
        self.dma('sp', lambda e: e.dma_start(out=out, in_=in_), reads, writes)

    def ST(self, out, in_, reads, writes=()):
        self.dma('sp', lambda e: e.dma_start(out=out, in_=in_), reads, writes)


def _consts():
    c = {}
    c['ident'] = np.eye(128, dtype=np.float32)
    j = np.arange(64)[:, None]
    t = np.arange(64)[None, :]
    m = np.zeros((64, 3, 64), np.float32)
    m[:, 0, :] = (j < t)
    m[:, 1, :] = (j <= t)
    m[:, 2, :] = (j > t)
    c['masks'] = m
    p = np.arange(128)
    c['blockones'] = (p[:, None] // 64 == p[None, :] // 64).astype(np.float32)
    hs = np.zeros((128, 2), np.float32)
    hs[:64, 0] = 1
    hs[64:, 1] = 1
    c['headsel'] = hs
    rm = np.ones((128, 512), np.float32)
    rm[:, ::64] = 0
    c['resetmask'] = rm
    return c


RW_GN_EPS = 64e-5


def build_rwkv(nblk):
    nc = bass.Bass("TRN2", target_bir_lowering=False)
    T = nblk * 512

    def din(name, shape):
        return nc.dram_tensor(name, shape, F32, kind="ExternalInput").ap()
    xT = din("xT", [128, 8, T + 1])
    wrkv = din("wrkv", [128, 3, 8, 128])
    mu = din("mu", [128, 6, 8])
    w1 = din("w1", [128, 8, 64])
    a1 = din("a1", [128, 8, 64])
    g1 = din("g1", [128, 8, 160])
    w2 = din("w2", [64, 128])
    a2 = din("a2", [64, 128])
    g2a = din("g2a", [128, 128])
    g2b = din("g2b", [32, 128])
    pcol = din("pcol", [128, 8])
    lnx = din("lnx", [64, 2, 128])
    c_ident = din("ident", [128, 128])
    c_masks = din("masks", [64, 3, 64])
    c_bo = din("blockones", [128, 128])
    c_hs = din("headsel", [128, 2])
    c_rm = din("resetmask", [128, 512])
    y = nc.dram_tensor("y", [T, 128], F32, kind="ExternalOutput").ap()

    P = Prog(nc)
    with ExitStack() as st:
        def sb(name, shape):
            return st.enter_context(nc.sbuf_tensor(name, shape, F32))

        def ps(name, shape):
            return st.enter_context(nc.psum_tensor(name, shape, F32))
        ident = sb("ident_s", [128, 128])
        masks = sb("masks_s", [64, 3, 64])
        bo = sb("bo_s", [128, 128])
        hs = sb("hs_s", [128, 2])
        rm = sb("rm_s", [128, 512])
        pc = sb("pc_s", [128, 8])
        lnx_s = sb("lnx_s", [64, 2, 128])
        mu_s = sb("mu_s", [128, 6, 8])
        omu_s = sb("omu_s", [128, 6, 8])
        wraw = sb("wraw", [128, 8, 160])
        W1 = [sb("W1_%d" % p, [128, 8, 128]) for p in range(3)]
        W2 = [sb("W2_%d" % p, [128, 8, 128]) for p in range(3)]
        L1 = [sb("L1_%d" % p, [128, 8, 160 if p == 2 else 64]) for p in range(3)]
        L2 = [sb("L2_%d" % p, [128, 8, 160 if p == 2 else 64]) for p in range(3)]
        w2s = sb("w2s", [64, 128])
        a2s = sb("a2s", [64, 128])
        g2as = sb("g2as", [128, 128])
        g2bs = sb("g2bs", [32, 128])
        xb = sb("xb", [128, 8, 513])
        fm = {n: sb("fm_" + n, [128, 512]) for n in
              ['r', 'k', 'v', 'g', 'h1', 'h2', 'ld', 'a', 'kk', 'kh', 'b', 'cl', 't1', 't2',
               'at', 'rt', 'bt', 'kt', 'bG', 'kG', 'q']}
        gam = sb("gam", [128, 8])
        tok = sb("tok", [64, 5, 8, 128])
        stok = sb("stok", [64, 2, 8])
        sc = {n: sb("sc_" + n, [64, 16, 64]) for n in ['N', 'NT', 'AakT', 'ArbT', 'ArkT', 'Pa', 'PTa', 'Ta', 'Tb']}
        WT = sb("WT", [128, 8, 64])
        Usb = sb("Usb", [64, 8, 2, 64])
        Yb = sb("Yb", [64, 16, 64])
        Yt = sb("Yt", [64, 16, 64])
        st16 = {n: sb("st16_" + n, [64, 16]) for n in ['s', 'm', 'q', 'r']}
        STs = [sb("ST0", [128, 64]), sb("ST1", [128, 64])]
        psA = ps("psA", [128, 1024])
        psB = ps("psB", [128, 1024])
        psC = ps("psC", [128, 1024])
        ps6 = ps("ps6", [128, 512])
        ps7 = ps("ps7", [128, 512])

        P.LD(ident[:], c_ident, ['ident'])
        P.LD(masks[:], c_masks, ['masks'])
        P.LD(bo[:], c_bo, ['bo'])
        P.LD(hs[:], c_hs, ['hs'])
        P.LD(rm[:], c_rm, ['rm'])
        P.LD(pc[:], pcol, ['pc'])
        P.LD(lnx_s[:], lnx, ['lnx'])
        P.LD(mu_s[:], mu, ['mu'])
        P.LD(w2s[:], w2, ['w2s'])
        P.LD(a2s[:], a2, ['a2s'])
        P.LD(g2as[:], g2a, ['g2as'])
        P.LD(g2bs[:], g2b, ['g2bs'])
        P.TS(omu_s[:], mu_s[:], -1.0, 1.0, ALU.mult, ALU.add, ['mu'], ['omu'])
        P.TS(pc[:, 4:5], pc[:, 3:4], -1.0, 1.0, ALU.mult, ALU.add, ['pc'], ['pc'])
        for p in range(3):
            P.LD(wraw[:, :, 0:128], wrkv[:, p], ['wraw'])
            for kc in range(8):
                P.TS(W1[p][:, kc, :], wraw[:, kc, 0:128], omu_s[:, p, kc:kc + 1], None, ALU.mult, None, ['wraw', 'omu'], ['W'])
                P.TS(W2[p][:, kc, :], wraw[:, kc, 0:128], mu_s[:, p, kc:kc + 1], None, ALU.mult, None, ['wraw', 'mu'], ['W'])
        for p, src in enumerate([w1, a1, g1]):
            n = 160 if p == 2 else 64
            P.LD(wraw[:, :, 0:n], src, ['wraw'])
            for kc in range(8):
                P.TS(L1[p][:, kc, :], wraw[:, kc, 0:n], omu_s[:, 3 + p, kc:kc + 1], None, ALU.mult, None, ['wraw', 'omu'], ['W'])
                P.TS(L2[p][:, kc, :], wraw[:, kc, 0:n], mu_s[:, 3 + p, kc:kc + 1], None, ALU.mult, None, ['wraw', 'mu'], ['W'])
        P.op('pool', lambda e: e.memset(STs[0][:], 0.0), [], ['ST0'])

        half = [0]

        def proj(Wa, Wb, msl, M, evac):
            h = half[0]
            half[0] ^= 1
            res = 'psA%d' % h
            out = psA[0:M, h * 512:(h + 1) * 512]
            for kc in range(8):
                P.MM(out, Wa[:, kc, msl], xb[:, kc, 1:513], kc == 0, False, ['W', 'xb'], [res])
                P.MM(out, Wb[:, kc, msl], xb[:, kc, 0:512], False, kc == 7, ['W', 'xb'], [res])
            evac(out, res)

        def small_mm(lhsT_list, rhs_list, rd, evac):
            h = half[0]
            half[0] ^= 1
            res = 'psA%d' % h
            out = psA[:, h * 512:(h + 1) * 512]
            n = len(lhsT_list)
            for i in range(n):
                P.MM(out, lhsT_list[i], rhs_list[i], i == 0, i == n - 1, rd, [res])
            evac(out, res)

        for blk in range(nblk):
            P.LD(xb[:], xT[:, :, blk * 512: blk * 512 + 513], ['xb'])
            for p, nm in enumerate(['r', 'k', 'v']):
                proj(W1[p], W2[p], slice(0, 128), 128,
                     lambda out, res, nm=nm: P.CP(fm[nm][:], out, [res], [nm], eng='act'))
            proj(L1[0], L2[0], slice(0, 64), 64,
                 lambda out, res: P.ACT(fm['h1'][0:64, :], out, AF.Tanh, [res], ['h1']))
            small_mm([w2s[:]], [fm['h1'][0:64, :]], ['w2s', 'h1'],
                     lambda out, res: P.ACT(fm['t1'][:], out, AF.Sigmoid, [res, 'pc'], ['t1'], bias=pc[:, 0:1]))
            P.TS(fm['ld'][:], fm['t1'][:], -0.6065306597126334, None, ALU.mult, None, ['t1'], ['ld'])
            proj(L1[1], L2[1], slice(0, 64), 64,
                 lambda out, res: P.CP(fm['h1'][0:64, :], out, [res], ['h1'], eng='act'))
            small_mm([a2s[:]], [fm['h1'][0:64, :]], ['a2s', 'h1'],
                     lambda out, res: P.ACT(fm['a'][:], out, AF.Sigmoid, [res, 'pc'], ['a'], bias=pc[:, 1:2]))
            proj(L1[2], L2[2], slice(0, 128), 128,
                 lambda out, res: P.ACT(fm['h1'][:], out, AF.Sigmoid, [res], ['h1']))
            proj(L1[2], L2[2], slice(128, 160), 32,
                 lambda out, res: P.ACT(fm['h2'][0:32, :], out, AF.Sigmoid, [res], ['h2']))
            small_mm([g2as[:], g2bs[:]], [fm['h1'][:], fm['h2'][0:32, :]], ['g2as', 'g2bs', 'h1', 'h2'],
                     lambda out, res: P.CP(fm['g'][:], out, [res], ['g'], eng='act'))
            P.TS(fm['t1'][:], fm['k'][:], pc[:, 2:3], None, ALU.mult, None, ['k', 'pc'], ['t1'])
            P.TT(fm['t2'][:], fm['t1'][:], fm['t1'][:], ALU.mult, ['t1'], ['t2'], eng='pool')
            small_mm([bo[:]], [fm['t2'][:]], ['bo', 't2'],
                     lambda out, res: P.ACT(fm['t2'][:], out, AF.Sqrt, [res], ['t2']))
            P.TS(fm['t2'][:], fm['t2'][:], 1e-12, None, ALU.max, None, ['t2'], ['t2'])
            P.RCP(fm['t2'][:], fm['t2'][:], ['t2'], ['t2'])
            P.TT(fm['kk'][:], fm['t1'][:], fm['t2'][:], ALU.mult, ['t1', 't2'], ['kk'])
            P.TS(fm['t1'][:], fm['a'][:], pc[:, 3:4], pc[:, 4:5], ALU.mult, ALU.add, ['a', 'pc'], ['t1'])
            P.TT(fm['kh'][:], fm['k'][:], fm['t1'][:], ALU.mult, ['k', 't1'], ['kh'])
            P.TT(fm['b'][:], fm['kk'][:], fm['a'][:], ALU.mult, ['kk', 'a'], ['b'], eng='pool')
            P.op('dve', lambda e: e.tensor_tensor_scan(out=fm['cl'][:], data0=rm[:], data1=fm['ld'][:], initial=0.0,
                                                       op0=ALU.mult, op1=ALU.add), ['rm', 'ld'], ['cl'])
            P.STT(fm['q'][:], fm['r'][:], pc[:, 5:6], fm['kh'][:], ALU.mult, ALU.mult, ['r', 'pc', 'kh'], ['q'])
            P.ACT(fm['t1'][:], fm['cl'][:], AF.Exp, ['cl'], ['t1'])
            P.TT(fm['rt'][:], fm['r'][:], fm['t1'][:], ALU.mult, ['r', 't1'], ['rt'])
            P.CP(gam[:], fm['t1'][:].rearrange("p (c t) -> p c t", t=64)[:, :, 63], ['t1'], ['gam'])
            P.ACT(fm['t2'][:], fm['cl'][:], AF.Exp, ['cl'], ['t2'], scale=-1.0)
            P.TT(fm['bt'][:], fm['b'][:], fm['t2'][:], ALU.mult, ['b', 't2'], ['bt'])
            P.TT(fm['kt'][:], fm['kh'][:], fm['t2'][:], ALU.mult, ['kh', 't2'], ['kt'], eng='pool')
            P.TT(fm['t1'][:], fm['cl'][:], fm['ld'][:], ALU.subtract, ['cl', 'ld'], ['t1'])
            P.ACT(fm['t1'][:], fm['t1'][:], AF.Exp, ['t1'], ['t1'])
            P.STT(fm['at'][:], fm['kk'][:], -1.0, fm['t1'][:], ALU.mult, ALU.mult, ['kk', 't1'], ['at'])
            clv = fm['cl'][:].rearrange("p (c t) -> p c t", t=64)
            P.TT(fm['t2'][:].rearrange("p (c t) -> p c t", t=64), clv[:, :, 63:64].to_broadcast([128, 8, 64]), clv,
                 ALU.subtract, ['cl'], ['t2'])
            P.ACT(fm['t2'][:], fm['t2'][:], AF.Exp, ['t2'], ['t2'])
            P.TT(fm['bG'][:], fm['b'][:], fm['t2'][:], ALU.mult, ['b', 't2'], ['bG'])
            P.TT(fm['kG'][:], fm['kh'][:], fm['t2'][:], ALU.mult, ['kh', 't2'], ['kG'], eng='pool')
            for c in range(8):
                pt = psB if c % 2 == 0 else psC
                pres = 'psB' if c % 2 == 0 else 'psC'
                cs = slice(c * 64, (c + 1) * 64)
                for i, nm in enumerate(['at', 'bG', 'kG', 'v', 'g']):
                    P.TR(pt[0:64, i * 128:(i + 1) * 128], fm[nm][:, cs], ident[:], [nm], [pres])
                P.MM(pt[0:64, 640:642], fm['q'][:, cs], hs[:], True, True, ['q', 'hs'], [pres])
                P.CP(tok[:, :, c, :], pt[0:64, 0:640].rearrange("p (i f) -> p i f", f=128), [pres], ['tok'],
                     eng='act' if c % 2 == 0 else 'dve')
                P.CP(stok[:, :, c], pt[0:64, 640:642], [pres], ['stok'])
            specs = [('NT', 'bt', 'at', 0), ('N', 'at', 'bt', 2), ('AakT', 'kt', 'at', 0),
                     ('ArbT', 'bt', 'rt', 1), ('ArkT', 'kt', 'rt', 1)]
            for si, (nm, l, r_, mi) in enumerate(specs):
                pt = psB if si % 2 == 0 else psC
                pres = 'psB' if si % 2 == 0 else 'psC'
                for c in range(8):
                    cs = slice(c * 64, (c + 1) * 64)
                    for h in range(2):
                        hp = slice(h * 64, (h + 1) * 64)
                        ch = h * 8 + c
                        P.MM(pt[0:64, ch * 64:(ch + 1) * 64], fm[l][hp, cs], fm[r_][hp, cs], True, True, [l, r_], [pres])
                P.TT(sc[nm][:], pt[0:64, :].rearrange("p (a b) -> p a b", b=64),
                     masks[:, mi:mi + 1, :].to_broadcast([64, 16, 64]), ALU.mult, [pres, 'masks'], [nm])
            P.TT(sc['Ta'][:], sc['NT'][:], ident[0:64, 0:64].unsqueeze(1).to_broadcast([64, 16, 64]), ALU.add,
                 ['NT', 'ident'], ['Ta'])
            Pn, PTn = 'N', 'NT'
            Tcur, Tnxt = 'Ta', 'Tb'
            for lvl in range(5):
                Pd, PTd = ('Pa', 'PTa') if lvl % 2 == 0 else ('N', 'NT')
                for ch in range(16):
                    P.MM(psB[0:64, ch * 64:(ch + 1) * 64], sc[PTn][:, ch, :], sc[Pn][:, ch, :], True, True, [PTn, Pn], ['psB'])
                for ch in range(16):
                    P.MM(psC[0:64, ch * 64:(ch + 1) * 64], sc[Pn][:, ch, :], sc[PTn][:, ch, :], True, True, [PTn, Pn], ['psC'])
                P.CP(sc[Pd][:], psB[0:64, :].rearrange("p (a b) -> p a b", b=64), ['psB'], [Pd], eng='act')
                P.CP(sc[PTd][:], psC[0:64, :].rearrange("p (a b) -> p a b", b=64), ['psC'], [PTd])
                Pn, PTn = Pd, PTd
                for ch in range(16):
                    P.MM(psB[0:64, ch * 64:(ch + 1) * 64], sc[Pn][:, ch, :], sc[Tcur][:, ch, :], True, True, [Pn, Tcur], ['psB'])
                P.TT(sc[Tnxt][:], psB[0:64, :].rearrange("p (a b) -> p a b", b=64), sc[Tcur][:], ALU.add, ['psB', Tcur], [Tnxt])
                Tcur, Tnxt = Tnxt, Tcur
            Ti = Tcur
            for c in range(8):
                for h in range(2):
                    ch = h * 8 + c
                    hp = slice(h * 64, (h + 1) * 64)
                    P.MM(psC[hp, c * 64:(c + 1) * 64], tok[:, 0, c, hp], sc[Ti][:, ch, :], True, True, ['tok', Ti], ['psC'])
                    P.MM(psB[0:64, ch * 64:(ch + 1) * 64], sc['AakT'][:, ch, :], tok[:, 3, c, hp], True, True, ['AakT', 'tok'], ['psB'])
            P.CP(WT[:], psC[:, 0:512].rearrange("p (c t) -> p c t", t=64), ['psC'], ['WT'], eng='act')
            P.CP(sc['N'][:], psB[0:64, :].rearrange("p (a b) -> p a b", b=64), ['psB'], ['N'])
            for ch in range(16):
                P.MM(psB[0:64, ch * 64:(ch + 1) * 64], sc[Ti][:, ch, :], sc['N'][:, ch, :], True, True, [Ti, 'N'], ['psB'])
            P.CP(sc['NT'][:], psB[0:64, :].rearrange("p (a b) -> p a b", b=64), ['psB'], ['NT'])
            psUh = [psA[0:64, 0:64], psA[0:64, 512:576]]
            psUn = ['psA0', 'psA1']
            psYh = [ps6[0:64, 0:64], ps7[0:64, 0:64]]
            psYn = ['ps6', 'ps7']
            for c in range(8):
                gi = blk * 8 + c
                Sc, Sn = STs[gi % 2], STs[(gi + 1) % 2]
                Scn, Snn = 'ST%d' % (gi % 2), 'ST%d' % ((gi + 1) % 2)
                cs = slice(c * 64, (c + 1) * 64)
                for h in range(2):
                    hp = slice(h * 64, (h + 1) * 64)
                    P.MM(psUh[h], WT[hp, c, :], Sc[hp, :], True, True, ['WT', Scn], [psUn[h]])
                for h in range(2):
                    P.TT(Usb[:, c, h, :], psUh[h], sc['NT'][:, h * 8 + c, :], ALU.add, [psUn[h], 'NT'], ['Usb'])
                for h in range(2):
                    hp = slice(h * 64, (h + 1) * 64)
                    ch = h * 8 + c
                    yo = psYh[h]
                    P.MM(yo, fm['rt'][hp, cs], Sc[hp, :], True, False, ['rt', Scn], [psYn[h]])
                    P.MM(yo, sc['ArbT'][:, ch, :], Usb[:, c, h, :], False, False, ['ArbT', 'Usb'], [psYn[h]])
                    P.MM(yo, sc['ArkT'][:, ch, :], tok[:, 3, c, hp], False, True, ['ArkT', 'tok'], [psYn[h]])
                for h in range(2):
                    hp = slice(h * 64, (h + 1) * 64)
                    so = psC[hp, 0:64]
                    P.MM(so, tok[:, 1, c, hp], Usb[:, c, h, :], True, False, ['tok', 'Usb'], ['psC'])
                    P.MM(so, tok[:, 2, c, hp], tok[:, 3, c, hp], False, True, ['tok'], ['psC'])
                P.STT(Sn[:], Sc[:], gam[:, c:c + 1], psC[:, 0:64], ALU.mult, ALU.add, [Scn, 'gam', 'psC'], [Snn])
                for h in range(2):
                    P.CP(Yb[:, h * 8 + c, :], psYh[h], [psYn[h]], ['Yb'], eng='act')
            P.RED(st16['s'][:], Yb[:], ALU.add, ['Yb'], ['s16'])
            P.TS(st16['m'][:], st16['s'][:], -1.0 / 64, None, ALU.mult, None, ['s16'], ['m16'])
            P.TT(Yb[:], Yb[:], st16['m'][:].unsqueeze(2).to_broadcast([64, 16, 64]), ALU.add, ['Yb', 'm16'], ['Yb'])
            P.TT(Yt[:], Yb[:], Yb[:], ALU.mult, ['Yb'], ['Yt'], eng='pool')
            P.RED(st16['q'][:], Yt[:], ALU.add, ['Yt'], ['q16'])
            P.ACT(st16['q'][:], st16['q'][:], AF.Sqrt, ['q16', 'pc'], ['q16'], bias=pc[0:64, 6:7], scale=1.0 / 64)
            P.RCP(st16['r'][:], st16['q'][:], ['q16'], ['r16'])
            P.TT(Yb[:], Yb[:], st16['r'][:].unsqueeze(2).to_broadcast([64, 16, 64]), ALU.mult, ['Yb', 'r16'], ['Yb'])
            Yb4 = Yb[:].rearrange("p (h c) v -> p h c v", h=2)
            Yt4 = Yt[:].rearrange("p (h c) v -> p h c v", h=2)
            lg = lnx_s[:, 0, :].rearrange("p (h v) -> p h v", h=2).unsqueeze(2).to_broadcast([64, 2, 8, 64])
            lb = lnx_s[:, 1, :].rearrange("p (h v) -> p h v", h=2).unsqueeze(2).to_broadcast([64, 2, 8, 64])
            P.TT(Yb4, Yb4, lg, ALU.mult, ['Yb', 'lnx'], ['Yb'])
            P.TT(Yb4, Yb4, lb, ALU.add, ['Yb', 'lnx'], ['Yb'])
            vt4 = tok[:, 3, :, :].rearrange("p c (h v) -> p h c v", h=2)
            gt4 = tok[:, 4, :, :].rearrange("p c (h v) -> p h c v", h=2)
            P.TT(Yt4, vt4, stok[:].unsqueeze(3).to_broadcast([64, 2, 8, 64]), ALU.mult, ['tok', 'stok'], ['Yt'])
            P.TT(Yb[:], Yb[:], Yt[:], ALU.add, ['Yb', 'Yt'], ['Yb'])
            P.TT(Yt4, Yb4, gt4, ALU.mult, ['Yb', 'tok'], ['Yt'])
            P.ST(y[blk * 512:(blk + 1) * 512, :].rearrange("(c t) (h v) -> t h c v", t=64, h=2), Yt4, ['Yt'])
        P.emit()
    return nc


def rwkv_inputs(hT_pad, j, core, inp):
    cs = slice(core * 128, (core + 1) * 128)
    f = np.float32

    def kl(w):
        return np.ascontiguousarray(w.reshape(8, 128, -1).transpose(1, 0, 2))
    d = {}
    d['xT'] = hT_pad
    d['wrkv'] = np.ascontiguousarray(np.stack([kl(inp['rw_w_rkv'][j, p][:, cs]) for p in range(3)], 1))
    d['mu'] = np.ascontiguousarray(inp['rw_mu'][j].reshape(6, 8, 128).transpose(2, 0, 1))
    d['w1'] = kl(inp['rw_w1'][j])
    d['a1'] = kl(inp['rw_a1'][j])
    d['g1'] = kl(inp['rw_g1'][j])
    d['w2'] = np.ascontiguousarray(inp['rw_w2'][j][:, cs])
    d['a2'] = np.ascontiguousarray(inp['rw_a2'][j][:, cs])
    d['g2a'] = np.ascontiguousarray(inp['rw_g2'][j][0:128, cs])
    d['g2b'] = np.ascontiguousarray(inp['rw_g2'][j][128:160, cs])
    pcol = np.zeros((128, 8), f)
    pcol[:, 0] = inp['rw_w0'][j][cs]
    pcol[:, 1] = inp['rw_a0'][j][cs]
    pcol[:, 2] = inp['rw_k_k'][j][cs]
    pcol[:, 3] = inp['rw_k_a'][j][cs]
    pcol[:, 5] = inp['rw_r_k'][j].reshape(-1)[cs]
    pcol[:, 6] = RW_GN_EPS
    d['pcol'] = pcol
    lnx = np.zeros((64, 2, 128), f)
    lnx[:, 0, :] = inp['rw_lnx_g'][j][cs][None, :]
    lnx[:, 1, :] = inp['rw_lnx_b'][j][cs][None, :]
    d['lnx'] = lnx
    d.update(_consts())
    return d


LN_EPS = 1e-5
TPC = SEQ // NCORES
HALF = 1024
NT_H = HALF // 128
CAP = 128


def build_post(kind):
    nk = {'rwkv': 8, 'mamba': 16, 'gla': 8}[kind]
    G = {'rwkv': 1, 'mamba': 4, 'gla': 4}[kind]
    kpg = nk // G
    nc = bass.Bass("TRN2", target_bir_lowering=False)

    def din(name, shape):
        return nc.dram_tensor(name, shape, F32, kind="ExternalInput").ap()
    h_in = din("h", [TPC, D])
    YT = din("YT", [128, nk, TPC])
    wout = din("wout", [128, nk, D])
    ngc = din("ngc", [128, nk])
    ssq_in = din("ssq", [TPC, 8])
    epsc = din("epsc", [128, 4])
    lnp = din("lnp", [128, 4, D])
    wr = din("wr", [128, 8, 36])
    br = din("br", [128, 36])
    w_in = din("w_in", [32, D, 1024])
    w_dn = din("w_dn", [32, 512, D])
    c_ident = din("ident", [128, 128])
    c_iota = din("iota", [128, 128])
    c_lt = din("strictlt", [128, 128])
    c_ones = din("ones", [128, 128])
    hout = nc.dram_tensor("hout", [TPC, D], F32, kind="ExternalOutput").ap()

    P = Prog(nc)
    with ExitStack() as st:
        def sb(name, shape):
            return st.enter_context(nc.sbuf_tensor(name, shape, F32))

        def ps(name, shape):
            return st.enter_context(nc.psum_tensor(name, shape, F32))
        ident = sb("ident_s", [128, 128])
        iota = sb("iota_s", [128, 128])
        slt = sb("slt_s", [128, 128])
        ones = sb("ones_s", [128, 128])
        eps_s = sb("eps_s", [128, 4])
        lnp_s = sb("lnp_s", [128, 4, D])
        wr_s = sb("wr_s", [128, 8, 36])
        br_s = sb("br_s", [128, 36])
        ngc_s = sb("ngc_s", [128, nk])
        ring = sb("ring", [128, 4, 4096])
        h1 = sb("h1", [128, NT_H, D])
        acc = sb("acc", [128, NT_H, D])
        yt = sb("yt", [128, nk, 128])
        sq = sb("sq", [128, nk, 128])
        tmp = sb("tmp", [128, D])
        tmp2 = sb("tmp2", [128, D])
        hT = sb("hT", [128, 8, 128])
        small = sb("small", [128, 64])
        ssq_s = sb("ssq_s", [128, 8])
        lg = sb("lg", [128, 36])
        ml = sb("ml", [128, 32])
        ml2 = sb("ml2", [128, 32])
        eqt = sb("eqt", [128, 32])
        Gt = sb("Gt", [128, NT_H, 32])
        selt = sb("selt", [128, NT_H, 32])
        posm = sb("posm", [128, NT_H, 32])
        Sel = sb("Sel", [128, NT_H, 128])
        SelT = sb("SelT", [128, NT_H, 128])
        xsT = sb("xsT", [128, 8, 128])
        gact = sb("gact", [128, 4, 128])
        AT = sb("AT", [128, 4, 128])
        Ys = sb("Ys", [128, D])
        gslot = sb("gslot", [128, 1])
        pa = ps("pa", [128, 1024])
        pb = ps("pb", [128, 1024])
        pc_ = ps("pc", [128, 1024])
        pd = ps("pd", [128, 1024])

        P.LD(ident[:], c_ident, ['ident'])
        P.LD(iota[:], c_iota, ['iota'])
        P.LD(slt[:], c_lt, ['slt'])
        P.LD(ones[:], c_ones, ['ones'])
        P.LD(eps_s[:], epsc, ['eps'])
        P.LD(lnp_s[:], lnp, ['lnp'])
        P.LD(wr_s[:], wr, ['wr'])
        P.LD(br_s[:], br, ['br'])
        P.LD(ngc_s[:], ngc, ['ngc'])

        def layer_norm(x_ap, xres, gi, out_ap, outres):
            P.RED(small[:, 0:1], x_ap, ALU.add, [xres], ['sm0'])
            P.TS(small[:, 1:2], small[:, 0:1], -1.0 / D, None, ALU.mult, None, ['sm0'], ['sm1'])
            P.TS(x_ap, x_ap, small[:, 1:2], None, ALU.add, None, [xres, 'sm1'], [xres])
            P.TT(tmp2[:], x_ap, x_ap, ALU.mult, [xres], ['tmp2'], eng='pool')
            P.RED(small[:, 2:3], tmp2[:], ALU.add, ['tmp2'], ['sm2'])
            P.ACT(small[:, 3:4], small[:, 2:3], AF.Sqrt, ['sm2', 'eps'], ['sm3'], bias=eps_s[:, 1:2], scale=1.0 / D)
            P.RCP(small[:, 4:5], small[:, 3:4], ['sm3'], ['sm4'])
            P.STT(tmp2[:], x_ap, small[:, 4:5], lnp_s[:, gi, :], ALU.mult, ALU.mult, [xres, 'sm4', 'lnp'], ['tmp2'])
            P.TT(out_ap, tmp2[:], lnp_s[:, gi + 1, :], ALU.add, ['tmp2', 'lnp'], [outres])

        for half in range(TPC // HALF):
            t0 = half * HALF
            for kc in range(nk):
                slot, off = divmod(kc * 1024, 4096)
                P.LD(ring[:, slot, off:off + 1024], wout[:, kc, :], ['ring%d' % slot])
                if kind != 'rwkv':
                    P.TS(ring[:, slot, off:off + 1024], ring[:, slot, off:off + 1024], ngc_s[:, kc:kc + 1], None,
                         ALU.mult, None, ['ring%d' % slot, 'ngc'], ['ring%d' % slot])
            for i in range(NT_H):
                ts = slice(t0 + i * 128, t0 + (i + 1) * 128)
                P.LD(yt[:], YT[:, :, ts], ['yt'])
                P.LD(tmp[:], h_in[ts, :], ['tmp'])
                if kind == 'mamba':
                    P.TT(sq[:], yt[:], yt[:], ALU.mult, ['yt'], ['sq'], eng='pool')
                    for g in range(G):
                        for k in range(kpg):
                            P.MM(pd[:, g:g + 1], sq[:, g * kpg + k, :], ones[:, 0:1], k == 0, k == kpg - 1, ['sq', 'ones'], ['pd'])
                    P.ACT(small[:, 8:12], pd[:, 0:4], AF.Sqrt, ['pd', 'eps'], ['sm8'], bias=eps_s[:, 0:1], scale=1.0 / 512)
                    P.RCP(small[:, 12:16], small[:, 8:12], ['sm8'], ['sm12'])
                elif kind == 'gla':
                    P.LD(ssq_s[:], ssq_in[ts, :], ['ssq'])
                    P.TT(small[:, 8:12], ssq_s[:, 0:4], ssq_s[:, 4:8], ALU.add, ['ssq'], ['sm8'])
                    P.ACT(small[:, 8:12], small[:, 8:12], AF.Sqrt, ['sm8', 'eps'], ['sm8'], bias=eps_s[:, 0:1], scale=1.0 / 256)
                    P.RCP(small[:, 12:16], small[:, 8:12], ['sm8'], ['sm12'])
                P.TS(tmp[:], tmp[:], ALPHA, None, ALU.mult, None, ['tmp'], ['tmp'])
                for g in range(G):
                    pp, pn = (pa, 'pa') if g % 2 == 0 else (pb, 'pb')
                    for hf in range(2):
                        for k in range(kpg):
                            kc = g * kpg + k
                            slot, off = divmod(kc * 1024, 4096)
                            P.MM(pp[:, hf * 512:(hf + 1) * 512], yt[:, kc, :], ring[:, slot, off + hf * 512: off + (hf + 1) * 512],
                                 k == 0, k == kpg - 1, ['yt', 'ring%d' % slot], [pn])
                    if kind == 'rwkv':
                        P.TT(tmp[:], tmp[:], pp[:], ALU.add, ['tmp', pn], ['tmp'])
                    else:
                        P.STT(tmp[:], pp[:], small[:, 12 + g:13 + g], tmp[:], ALU.mult, ALU.add, [pn, 'sm12', 'tmp'], ['tmp'])
                layer_norm(tmp[:], 'tmp', 0, h1[:, i, :], 'h1')
                P.TS(acc[:, i, :], h1[:, i, :], ALPHA, None, ALU.mult, None, ['h1'], ['acc'], eng='pool')
                for kc in range(8):
                    P.TR(pa[:, kc * 128:(kc + 1) * 128], h1[:, i, kc * 128:(kc + 1) * 128], ident[:], ['h1'], ['pa'])
                P.CP(hT[:], pa[:].rearrange("p (k t) -> p k t", t=128), ['pa'], ['hT'], eng='act')
                for kc in range(8):
                    P.MM(pb[:, 0:36], hT[:, kc, :], wr_s[:, kc, :], kc == 0, kc == 7, ['hT', 'wr'], ['pb'])
                P.TT(lg[:], pb[:, 0:36], br_s[:], ALU.add, ['pb', 'br'], ['lg'])
                P.RED(small[:, 16:17], lg[:, 0:4], ALU.max, ['lg'], ['sm16'])
                P.TS(small[:, 17:18], small[:, 16:17], -1.0, None, ALU.mult, None, ['sm16'], ['sm17'])
                P.ACT(small[:, 20:24], lg[:, 0:4], AF.Exp, ['lg', 'sm17'], ['sm20'], bias=small[:, 17:18])
                P.RED(small[:, 18:19], small[:, 20:24], ALU.add, ['sm20'], ['sm18'])
                P.RCP(small[:, 19:20], small[:, 18:19], ['sm18'], ['sm19'])
                P.TS(small[:, 24:28], lg[:, 0:4], small[:, 16:17], None, ALU.is_ge, None, ['lg', 'sm16'], ['sm24'])
                P.TS(small[:, 24:28], small[:, 24:28], -1.0, 1e9, ALU.add, ALU.mult, ['sm24'], ['sm24'])
                P.TT(ml[:].rearrange("p (g e) -> p g e", e=8), lg[:, 4:36].rearrange("p (g e) -> p g e", e=8),
                     small[:, 24:28].unsqueeze(2).to_broadcast([128, 4, 8]), ALU.add, ['lg', 'sm24'], ['ml'])
                P.RED(small[:, 28:29], ml[:], ALU.max, ['ml'], ['sm28'])
                P.TS(eqt[:], ml[:], small[:, 28:29], -1e9, ALU.is_ge, ALU.mult, ['ml', 'sm28'], ['eqt'])
                P.TT(ml2[:], ml[:], eqt[:], ALU.add, ['ml', 'eqt'], ['ml2'])
                P.RED(small[:, 29:30], ml2[:], ALU.max, ['ml2'], ['sm29'])
                P.TS(selt[:, i, :], ml[:], small[:, 29:30], None, ALU.is_ge, None, ['ml', 'sm29'], ['selt'])
                P.TS(small[:, 30:31], small[:, 28:29], -1.0, None, ALU.mult, None, ['sm28'], ['sm30'])
                P.ACT(ml2[:], ml[:], AF.Exp, ['ml', 'sm30'], ['ml2'], bias=small[:, 30:31])
                P.TT(ml2[:], ml2[:], selt[:, i, :], ALU.mult, ['ml2', 'selt'], ['ml2'])
                P.RED(small[:, 31:32], ml2[:], ALU.add, ['ml2'], ['sm31'])
                P.RCP(small[:, 32:33], small[:, 31:32], ['sm31'], ['sm32'])
                P.TS(Gt[:, i, :], ml2[:], small[:, 32:33], small[:, 19:20], ALU.mult, ALU.mult, ['ml2', 'sm32', 'sm19'], ['Gt'])
                P.MM(pc_[:, 0:32], slt[:], selt[:, i, :], True, i == 0, ['slt', 'selt'], ['pc'])
                for i2 in range(i):
                    P.MM(pc_[:, 0:32], ones[:], selt[:, i2, :], False, i2 == i - 1, ['ones', 'selt'], ['pc'])
                P.TS(eqt[:], selt[:, i, :], -1e6, 1e6, ALU.mult, ALU.add, ['selt'], ['eqt'])
                P.TT(posm[:, i, :], pc_[:, 0:32], eqt[:], ALU.add, ['pc', 'eqt'], ['posm'])
            piece = [half * 96]

            def load_piece(src_ap):
                slot = piece[0] % 4
                piece[0] += 1
                P.LD(ring[:, slot, :], src_ap, ['ring%d' % slot])
                return slot
            for e in range(32):
                P.TT(Sel[:], iota[:].unsqueeze(1).to_broadcast([128, NT_H, 128]),
                     posm[:, :, e:e + 1].to_broadcast([128, NT_H, 128]), ALU.is_equal, ['iota', 'posm'], ['Sel'])
                for kc in range(8):
                    for i in range(NT_H):
                        P.MM(pa[:, kc * 128:(kc + 1) * 128], h1[:, i, kc * 128:(kc + 1) * 128], Sel[:, i, :], i == 0, i == NT_H - 1,
                             ['h1', 'Sel'], ['pa'])
                P.CP(xsT[:], pa[:].rearrange("p (k t) -> p k t", t=128), ['pa'], ['xsT'], eng='act')
                for i in range(NT_H):
                    P.MM(pd[:, 1000:1001], Sel[:, i, :], Gt[:, i, e:e + 1], i == 0, i == NT_H - 1, ['Sel', 'Gt'], ['pd'])
                P.CP(gslot[:], pd[:, 1000:1001], ['pd'], ['gslot'])
                for i in range(NT_H):
                    P.TR(pd[:, i * 128:(i + 1) * 128], Sel[:, i, :], ident[:], ['Sel'], ['pd'])
                P.CP(SelT[:], pd[:, 0:NT_H * 128].rearrange("p (k t) -> p k t", t=128), ['pd'], ['SelT'], eng='act')
                sg = load_piece(w_in[e, :, 0:512].rearrange("(kc kp) n -> kp kc n", kp=128))
                for fc in range(4):
                    for kc in range(8):
                        P.MM(pb[:, fc * 128:(fc + 1) * 128], ring[:, sg, kc * 512 + fc * 128: kc * 512 + (fc + 1) * 128], xsT[:, kc, :],
                             kc == 0, kc == 7, ['ring%d' % sg, 'xsT'], ['pb'])
                P.ACT(gact[:], pb[:, 0:512].rearrange("p (k t) -> p k t", t=128), AF.Silu, ['pb'], ['gact'])
                su = load_piece(w_in[e, :, 512:1024].rearrange("(kc kp) n -> kp kc n", kp=128))
                for fc in range(4):
                    for kc in range(8):
                        P.MM(pb[:, 512 + fc * 128: 512 + (fc + 1) * 128], ring[:, su, kc * 512 + fc * 128: kc * 512 + (fc + 1) * 128],
                             xsT[:, kc, :], kc == 0, kc == 7, ['ring%d' % su, 'xsT'], ['pb'])
                P.TT(AT[:], gact[:], pb[:, 512:1024].rearrange("p (k t) -> p k t", t=128), ALU.mult, ['gact', 'pb'], ['AT'])
                sd = load_piece(w_dn[e].rearrange("(kc kp) n -> kp kc n", kp=128))
                for hf in range(2):
                    for fk in range(4):
                        P.MM(pc_[:, hf * 512:(hf + 1) * 512], AT[:, fk, :], ring[:, sd, fk * 1024 + hf * 512: fk * 1024 + (hf + 1) * 512],
                             fk == 0, fk == 3, ['AT', 'ring%d' % sd], ['pc'])
                P.TS(Ys[:], pc_[:], gslot[:, 0:1], None, ALU.mult, None, ['pc', 'gslot'], ['Ys'])
                for i in range(NT_H):
                    pp, pn = (pa, 'pa') if i % 2 == 0 else (pb, 'pb')
                    for hf in range(2):
                        P.MM(pp[:, hf * 512:(hf + 1) * 512], SelT[:, i, :], Ys[:, hf * 512:(hf + 1) * 512], True, True, ['SelT', 'Ys'], [pn])
                    P.TT(acc[:, i, :], acc[:, i, :], pp[:], ALU.add, ['acc', pn], ['acc'])
            for i in range(NT_H):
                layer_norm(acc[:, i, :], 'acc', 2, tmp[:], 'tmp')
                P.ST(hout[t0 + i * 128: t0 + (i + 1) * 128, :], tmp[:], ['tmp'])
        P.emit()
    return nc


def _post_consts():
    c = {}
    c['ident'] = np.eye(128, dtype=np.float32)
    c['iota'] = np.broadcast_to(np.arange(128, dtype=np.float32)[None, :], (128, 128)).copy()
    a = np.arange(128)
    c['strictlt'] = (a[:, None] < a[None, :]).astype(np.float32)
    c['ones'] = np.ones((128, 128), np.float32)
    return c


def kl(w):
    return np.ascontiguousarray(w.reshape(-1, 128, w.shape[-1]).transpose(1, 0, 2))


def post_inputs(kind, layer, core, h, YT_full, inp, ngvec=None, ssq=None, mix_eps=1e-5):
    f = np.float32
    ts = slice(core * TPC, (core + 1) * TPC)
    d = {}
    d['h'] = np.ascontiguousarray(h[ts])
    nk = YT_full.shape[0] // 128
    d['YT'] = np.ascontiguousarray(YT_full[:, ts].reshape(nk, 128, TPC).transpose(1, 0, 2))
    j = layer // 3
    wo = {'rwkv': lambda: inp['rw_w_o'][j], 'mamba': lambda: inp['mb_w_out'][j], 'gla': lambda: inp['gl_w_out'][j]}[kind]()
    d['wout'] = kl(wo)
    if ngvec is None:
        ngvec = np.ones(nk * 128, f)
    d['ngc'] = np.ascontiguousarray(ngvec.reshape(nk, 128).T)
    d['ssq'] = np.ascontiguousarray(ssq[ts]) if ssq is not None else np.zeros((TPC, 8), f)
    ec = np.zeros((128, 4), f)
    ec[:, 0] = mix_eps
    ec[:, 1] = LN_EPS
    d['epsc'] = ec
    lnp = np.stack([inp['ln_g'][layer, 0], inp['ln_b'][layer, 0], inp['ln_g'][layer, 1], inp['ln_b'][layer, 1]], 0)
    d['lnp'] = np.ascontiguousarray(np.broadcast_to(lnp[None], (128, 4, D)))
    wrt = np.concatenate([inp['moe_w_group'][layer], inp['moe_w_route'][layer]], axis=1)
    d['wr'] = kl(wrt)
    brt = np.concatenate([inp['moe_b_group'][layer], inp['moe_b_route'][layer]], axis=0)
    d['br'] = np.ascontiguousarray(np.broadcast_to(brt[None], (128, 36)))
    d['w_in'] = inp['moe_w_in'][layer]
    d['w_dn'] = inp['moe_w_down'][layer]
    d.update(_post_consts())
    return d


def build_gla(nblk):
    nc = bass.Bass("TRN2", target_bir_lowering=False)
    T = nblk * 512

    def din(name, shape):
        return nc.dram_tensor(name, shape, F32, kind="ExternalInput").ap()
    xT = din("xT", [128, 8, T])
    wq = din("wqkvg", [128, 4, 8, 128])
    wlr = din("wlr", [128, 8, 16])
    gkw2 = din("gkw2", [16, 128])
    pcol = din("pcol", [128, 4])
    c_ident = din("ident", [128, 128])
    c_masks = din("masks", [64, 3, 64])
    c_rm = din("resetmask", [128, 512])
    y = nc.dram_tensor("y", [T, 128], F32, kind="ExternalOutput").ap()
    ssq = nc.dram_tensor("ssq", [64, nblk * 8], F32, kind="ExternalOutput").ap()

    P = Prog(nc)
    with ExitStack() as st:
        def sb(name, shape):
            return st.enter_context(nc.sbuf_tensor(name, shape, F32))

        def ps(name, shape):
            return st.enter_context(nc.psum_tensor(name, shape, F32))
        ident = sb("ident_s", [128, 128])
        masks = sb("masks_s", [64, 3, 64])
        rm = sb("rm_s", [128, 512])
        pc = sb("pc_s", [128, 4])
        W = sb("W", [128, 4, 8, 128])
        Wlr = sb("Wlr", [128, 8, 16])
        w2s = sb("w2s", [16, 128])
        xb = sb("xb", [128, 8, 512])
        fm = {n: sb("fm_" + n, [128, 512]) for n in ['q', 'k', 'v', 'sg', 'lr', 'la', 'cl', 't1', 't2', 'qt', 'kt', 'kG']}
        gam = sb("gam", [128, 8])
        tok = sb("tok", [64, 3, 8, 128])
        ArkT = sb("ArkT", [64, 8, 64])
        Yb = sb("Yb", [64, 8, 128])
        Yt = sb("Yt", [64, 8, 128])
        ssq_s = sb("ssq_s", [64, nblk * 8])
        STs = [sb("ST0", [128, 128]), sb("ST1", [128, 128])]
        psA = ps("psA", [128, 1024])
        psB = ps("psB", [128, 1024])
        psC = ps("psC", [128, 512])
        psY = ps("psY", [128, 512])
        psN = ps("psN", [128, 512])

        P.LD(ident[:], c_ident, ['ident'])
        P.LD(masks[:], c_masks, ['masks'])
        P.LD(rm[:], c_rm, ['rm'])
        P.LD(pc[:], pcol, ['pc'])
        P.LD(W[:], wq, ['W'])
        P.LD(Wlr[:], wlr, ['W'])
        P.LD(w2s[:], gkw2, ['w2s'])
        P.TS(pc[:, 1:2], pc[:, 0:1], -1.0, None, ALU.mult, None, ['pc'], ['pc'])
        P.op('pool', lambda e: e.memset(STs[0][:], 0.0), [], ['ST0'])
        half = [0]

        def proj(lhs_of_kc, M, evac):
            h = half[0]
            half[0] ^= 1
            res = 'psA%d' % h
            out = psA[0:M, h * 512:(h + 1) * 512]
            for kc in range(8):
                P.MM(out, lhs_of_kc(kc), xb[:, kc, :], kc == 0, kc == 7, ['W', 'xb'], [res])
            evac(out, res)

        for blk in range(nblk):
            P.LD(xb[:], xT[:, :, blk * 512:(blk + 1) * 512], ['xb'])
            proj(lambda kc: W[:, 0, kc, :], 128,
                 lambda out, res: P.ACT(fm['q'][:], out, AF.Copy, [res], ['q'], scale=128.0 ** -0.5))
            proj(lambda kc: W[:, 1, kc, :], 128, lambda out, res: P.CP(fm['k'][:], out, [res], ['k']))
            proj(lambda kc: W[:, 2, kc, :], 128, lambda out, res: P.CP(fm['v'][:], out, [res], ['v'], eng='act'))
            proj(lambda kc: W[:, 3, kc, :], 128, lambda out, res: P.ACT(fm['sg'][:], out, AF.Silu, [res], ['sg']))
            proj(lambda kc: Wlr[:, kc, :], 16, lambda out, res: P.CP(fm['lr'][0:16, :], out, [res], ['lr']))
            h = half[0]
            half[0] ^= 1
            res = 'psA%d' % h
            out = psA[:, h * 512:(h + 1) * 512]
            P.MM(out, w2s[:], fm['lr'][0:16, :], True, True, ['w2s', 'lr'], [res])
            P.ACT(fm['t1'][:], out, AF.Exp, [res, 'pc'], ['t1'], bias=pc[:, 1:2], scale=-1.0)
            P.ACT(fm['t1'][:], fm['t1'][:], AF.Ln, ['t1', 'pc'], ['t1'], bias=pc[:, 2:3], scale=1.0)
            P.TS(fm['la'][:], fm['t1'][:], -1.0 / 16.0, None, ALU.mult, None, ['t1'], ['la'])
            P.op('dve', lambda e: e.tensor_tensor_scan(out=fm['cl'][:], data0=rm[:], data1=fm['la'][:], initial=0.0,
                                                       op0=ALU.mult, op1=ALU.add), ['rm', 'la'], ['cl'])
            P.ACT(fm['t1'][:], fm['cl'][:], AF.Exp, ['cl'], ['t1'])
            P.TT(fm['qt'][:], fm['q'][:], fm['t1'][:], ALU.mult, ['q', 't1'], ['qt'])
            P.CP(gam[:], fm['t1'][:].rearrange("p (c t) -> p c t", t=64)[:, :, 63], ['t1'], ['gam'])
            P.ACT(fm['t2'][:], fm['cl'][:], AF.Exp, ['cl'], ['t2'], scale=-1.0)
            P.TT(fm['kt'][:], fm['k'][:], fm['t2'][:], ALU.mult, ['k', 't2'], ['kt'], eng='pool')
            clv = fm['cl'][:].rearrange("p (c t) -> p c t", t=64)
            P.TT(fm['t2'][:].rearrange("p (c t) -> p c t", t=64), clv[:, :, 63:64].to_broadcast([128, 8, 64]), clv,
                 ALU.subtract, ['cl'], ['t2'])
            P.ACT(fm['t2'][:], fm['t2'][:], AF.Exp, ['t2'], ['t2'])
            P.TT(fm['kG'][:], fm['k'][:], fm['t2'][:], ALU.mult, ['k', 't2'], ['kG'])
            for c in range(8):
                cs = slice(c * 64, (c + 1) * 64)
                for i, nm in enumerate(['kG', 'v', 'sg']):
                    P.TR(psB[0:64, i * 128:(i + 1) * 128], fm[nm][:, cs], ident[:], [nm], ['psB'])
                P.CP(tok[:, :, c, :], psB[0:64, 0:384].rearrange("p (i f) -> p i f", f=128), ['psB'], ['tok'],
                     eng='act' if c % 2 == 0 else 'dve')
            for c in range(8):
                cs = slice(c * 64, (c + 1) * 64)
                P.MM(psC[0:64, cs], fm['kt'][:, cs], fm['qt'][:, cs], True, True, ['kt', 'qt'], ['psC'])
            P.TT(ArkT[:], psC[0:64, :].rearrange("p (a b) -> p a b", b=64), masks[:, 1:2, :].to_broadcast([64, 8, 64]),
                 ALU.mult, ['psC', 'masks'], ['ArkT'])
            for c in range(8):
                gi = blk * 8 + c
                Sc, Sn = STs[gi % 2], STs[(gi + 1) % 2]
                Scn, Snn = 'ST%d' % (gi % 2), 'ST%d' % ((gi + 1) % 2)
                cs = slice(c * 64, (c + 1) * 64)
                P.MM(psY[0:64, 0:128], fm['qt'][:, cs], Sc[:], True, False, ['qt', Scn], ['psY'])
                P.MM(psY[0:64, 0:128], ArkT[:, c, :], tok[:, 1, c, :], False, True, ['ArkT', 'tok'], ['psY'])
                P.MM(psN[:, 0:128], tok[:, 0, c, :], tok[:, 1, c, :], True, True, ['tok'], ['psN'])
                P.STT(Sn[:], Sc[:], gam[:, c:c + 1], psN[:, 0:128], ALU.mult, ALU.add, [Scn, 'gam', 'psN'], [Snn])
                P.CP(Yb[:, c, :], psY[0:64, 0:128], ['psY'], ['Yb'], eng='act')
            P.TT(Yt[:], Yb[:], Yb[:], ALU.mult, ['Yb'], ['Yt'], eng='pool')
            P.RED(ssq_s[:, blk * 8:(blk + 1) * 8], Yt[:], ALU.add, ['Yt'], ['ssq'])
            P.TT(Yt[:], Yb[:], tok[:, 2, :, :], ALU.mult, ['Yb', 'tok'], ['Yt'])
            P.ST(y[blk * 512:(blk + 1) * 512, :].rearrange("(c t) f -> t c f", t=64), Yt[:], ['Yt'])
        P.ST(ssq, ssq_s[:], ['ssq'])
        P.emit()
    return nc


GL_KD, GL_VD = 512, 1024


def gla_inputs(hT, core, inp):
    j = 0
    hd, hf = core // 2, core % 2
    w = inp['gl_w_in'][j]
    qc = slice(hd * 128, (hd + 1) * 128)
    kc = slice(GL_KD + hd * 128, GL_KD + (hd + 1) * 128)
    v0 = 2 * GL_KD + hd * 256 + hf * 128
    g0 = 2 * GL_KD + GL_VD + hd * 256 + hf * 128
    d = {}
    d['xT'] = hT
    d['wqkvg'] = np.ascontiguousarray(np.stack([kl(w[:, qc]), kl(w[:, kc]), kl(w[:, v0:v0 + 128]), kl(w[:, g0:g0 + 128])], 1))
    d['wlr'] = kl(w[:, 2 * GL_KD + 2 * GL_VD:])
    d['gkw2'] = np.ascontiguousarray(inp['gl_gk_w2'][j][:, qc])
    pcol = np.zeros((128, 4), np.float32)
    pcol[:, 0] = inp['gl_gk_b'][j][qc]
    pcol[:, 2] = 1.0
    d['pcol'] = pcol
    c = _consts()
    d['ident'], d['masks'], d['resetmask'] = c['ident'], c['masks'], c['resetmask']
    return d


def build_mamba(nblk):
    nc = bass.Bass("TRN2", target_bir_lowering=False)
    T = nblk * 512

    def din(name, shape):
        return nc.dram_tensor(name, shape, F32, kind="ExternalInput").ap()
    xT = din("xT", [128, 8, T])
    wz = din("wz", [128, 2, 8, 128])
    wx = din("wx", [128, 4, 8, 128])
    wdt = din("wdt", [128, 8, 4])
    cw = din("cw", [128, 4, 5])
    pcol = din("pcol", [4, 4])
    dvec = din("dvec", [64, 4])
    c_ident = din("ident", [128, 128])
    c_masks = din("masks", [64, 3, 64])
    c_rm = din("resetmask", [128, 512])
    c_hsel = din("hsel", [4, 4, 64])
    c_eye4 = din("eye4", [4, 4])
    c_ones = din("ones", [128, 128])
    y = nc.dram_tensor("y", [T, 256], F32, kind="ExternalOutput").ap()

    P = Prog(nc)
    with ExitStack() as st:
        def sb(name, shape):
            return st.enter_context(nc.sbuf_tensor(name, shape, F32))

        def ps(name, shape):
            return st.enter_context(nc.psum_tensor(name, shape, F32))
        ident = sb("ident_s", [128, 128])
        masks = sb("masks_s", [64, 3, 64])
        rm = sb("rm_s", [128, 512])
        hsel = sb("hsel_s", [4, 4, 64])
        eye4 = sb("eye4_s", [4, 4])
        ones = sb("ones_s", [128, 128])
        pc = sb("pc_s", [4, 4])
        dv = sb("dv_s", [64, 4])
        cw_s = sb("cw_s", [128, 4, 5])
        Wz = sb("Wz", [128, 2, 8, 128])
        Wx = sb("Wx", [128, 4, 8, 128])
        Wdt = sb("Wdt", [128, 8, 4])
        xb = sb("xb", [128, 8, 512])
        pre4 = sb("pre4", [128, 4, 515])
        cv = sb("cv", [128, 4, 512])
        sz = sb("sz", [128, 2, 512])
        cacc = sb("cacc", [128, 512])
        r4 = {n: sb("r4_" + n, [4, 512]) for n in ['dt', 'dta', 'cl', 'ecl', 'dend', 't']}
        gamT = sb("gamT", [4, 8])
        Dg = sb("Dg", [4, 8, 4])
        gamB = sb("gamB", [128, 8, 4])
        tok = sb("tok", [64, 8, 640])
        tsm = sb("tsm", [64, 8, 16])
        xdt = sb("xdt", [64, 8, 256])
        xdtd = sb("xdtd", [64, 8, 256])
        scT = sb("scT", [64, 8, 64])
        tmpL = sb("tmpL", [64, 8, 64])
        ArkT = sb("ArkT", [64, 4, 8, 64])
        tmpY = sb("tmpY", [64, 256])
        Stmp = sb("Stmp", [128, 256])
        Yb = sb("Yb", [64, 8, 256])
        Yt = sb("Yt", [64, 8, 256])
        STs = [sb("ST0", [128, 256]), sb("ST1", [128, 256])]
        psA = ps("psA", [128, 1024])
        psB = ps("psB", [128, 1024])
        psC = ps("psC", [128, 512])
        psYi = ps("psYi", [128, 512])
        psYo = ps("psYo", [128, 512])
        psN = ps("psN", [128, 512])

        for (t_, s_, r_) in [(ident, c_ident, 'ident'), (masks, c_masks, 'masks'), (rm, c_rm, 'rm'), (hsel, c_hsel, 'hsel'),
                             (eye4, c_eye4, 'eye4'), (ones, c_ones, 'ones'), (pc, pcol, 'pc'), (dv, dvec, 'dv'),
                             (cw_s, cw, 'cw'), (Wz, wz, 'W'), (Wx, wx, 'W'), (Wdt, wdt, 'W')]:
            P.LD(t_[:], s_, [r_])
        P.ACT(pc[:, 3:4], pc[:, 1:2], AF.Exp, ['pc'], ['pc'])
        P.TS(pc[:, 3:4], pc[:, 3:4], -1.0, None, ALU.mult, None, ['pc'], ['pc'])
        P.op('pool', lambda e: e.memset(STs[0][:], 0.0), [], ['ST0'])
        P.op('pool', lambda e: e.memset(pre4[:], 0.0), [], ['pre4'])
        half = [0]

        def proj(lhs_of_kc, M, evac):
            h = half[0]
            half[0] ^= 1
            res = 'psA%d' % h
            out = psA[0:M, h * 512:(h + 1) * 512]
            for kc in range(8):
                P.MM(out, lhs_of_kc(kc), xb[:, kc, :], kc == 0, kc == 7, ['W', 'xb'], [res])
            evac(out, res)

        for blk in range(nblk):
            P.LD(xb[:], xT[:, :, blk * 512:(blk + 1) * 512], ['xb'])
            for i in range(2):
                proj(lambda kc, i=i: Wz[:, i, kc, :], 128,
                     lambda out, res, i=i: P.ACT(sz[:, i, :], out, AF.Silu, [res], ['sz']))
            for i in range(4):
                proj(lambda kc, i=i: Wx[:, i, kc, :], 128,
                     lambda out, res, i=i: P.CP(pre4[:, i, 3:515], out, [res], ['pre4'], eng='act' if i % 2 == 0 else 'dve'))
            proj(lambda kc: Wdt[:, kc, :], 4,
                 lambda out, res: P.ACT(r4['t'][:], out, AF.Exp, [res, 'pc'], ['r4t'], bias=pc[:, 0:1]))
            P.ACT(r4['dt'][:], r4['t'][:], AF.Ln, ['r4t', 'pc'], ['dt'], bias=pc[:, 2:3])
            for i in range(4):
                P.TS(cacc[:], pre4[:, i, 0:512], cw_s[:, i, 0:1], None, ALU.mult, None, ['pre4', 'cw'], ['cacc'])
                for tp in range(1, 4):
                    P.STT(cacc[:], pre4[:, i, tp:tp + 512], cw_s[:, i, tp:tp + 1], cacc[:], ALU.mult, ALU.add,
                          ['pre4', 'cw', 'cacc'], ['cacc'])
                P.ACT(cv[:, i, :], cacc[:], AF.Silu, ['cacc', 'cw'], ['cv'], bias=cw_s[:, i, 4:5])
            P.CP(pre4[:, :, 0:3], pre4[:, :, 512:515], ['pre4'], ['pre4'])
            P.TS(r4['dta'][:], r4['dt'][:], pc[:, 3:4], None, ALU.mult, None, ['dt', 'pc'], ['dta'])
            P.op('dve', lambda e: e.tensor_tensor_scan(out=r4['cl'][:], data0=rm[0:4, :], data1=r4['dta'][:], initial=0.0,
                                                       op0=ALU.mult, op1=ALU.add), ['rm', 'dta'], ['cl'])
            P.ACT(r4['ecl'][:], r4['cl'][:], AF.Exp, ['cl'], ['ecl'])
            P.CP(gamT[:], r4['ecl'][:].rearrange("p (c t) -> p c t", t=64)[:, :, 63], ['ecl'], ['gamT'])
            clv = r4['cl'][:].rearrange("p (c t) -> p c t", t=64)
            P.TT(r4['dend'][:].rearrange("p (c t) -> p c t", t=64), clv[:, :, 63:64].to_broadcast([4, 8, 64]), clv,
                 ALU.subtract, ['cl'], ['dend'])
            P.ACT(r4['dend'][:], r4['dend'][:], AF.Exp, ['dend'], ['dend'])
            P.TT(Dg[:], gamT[:].unsqueeze(2).to_broadcast([4, 8, 4]), eye4[:].unsqueeze(1).to_broadcast([4, 8, 4]), ALU.mult,
                 ['gamT', 'eye4'], ['Dg'])
            P.MM(psC[:, 0:32], ones[0:4, :], Dg[:].rearrange("p c h -> p (c h)"), True, True, ['ones', 'Dg'], ['psC'])
            P.CP(gamB[:], psC[:, 0:32].rearrange("p (c h) -> p c h", h=4), ['psC'], ['gamB'])
            for c in range(8):
                pt = psB
                cs = slice(c * 64, (c + 1) * 64)
                P.TR(pt[0:64, 0:128], cv[:, 0, cs], ident[:], ['cv'], ['psB'])
                P.TR(pt[0:64, 128:256], cv[:, 1, cs], ident[:], ['cv'], ['psB'])
                P.TR(pt[0:64, 256:384], cv[:, 2, cs], ident[:], ['cv'], ['psB'])
                P.TR(pt[0:64, 384:512], sz[:, 0, cs], ident[:], ['sz'], ['psB'])
                P.TR(pt[0:64, 512:640], sz[:, 1, cs], ident[:], ['sz'], ['psB'])
                for i, nm in enumerate(['dt', 'cl', 'ecl', 'dend']):
                    P.TR(pt[0:64, 640 + 4 * i: 644 + 4 * i], r4[nm][:, cs], ident[0:4, 0:4], [nm], ['psB'])
                P.CP(tok[:, c, :], pt[0:64, 0:640], ['psB'], ['tok'], eng='act')
                P.CP(tsm[:, c, :], pt[0:64, 640:656], ['psB'], ['tsm'])
            xs4 = tok[:, :, 0:256].rearrange("p c (h v) -> p c h v", h=4)
            P.TT(xdt[:].rearrange("p c (h v) -> p c h v", h=4), xs4, tsm[:, :, 0:4].unsqueeze(3).to_broadcast([64, 8, 4, 64]),
                 ALU.mult, ['tok', 'tsm'], ['xdt'])
            P.TT(xdtd[:].rearrange("p c (h v) -> p c h v", h=4), xdt[:].rearrange("p c (h v) -> p c h v", h=4),
                 tsm[:, :, 12:16].unsqueeze(3).to_broadcast([64, 8, 4, 64]), ALU.mult, ['xdt', 'tsm'], ['xdtd'])
            for c in range(8):
                cs = slice(c * 64, (c + 1) * 64)
                P.MM(psC[0:64, cs], cv[:, 2, cs], cv[:, 3, cs], True, True, ['cv'], ['psC'])
            P.TT(scT[:], psC[0:64, :].rearrange("p (a b) -> p a b", b=64), masks[:, 1:2, :].to_broadcast([64, 8, 64]),
                 ALU.mult, ['psC', 'masks'], ['scT'])
            for h in range(4):
                for c in range(8):
                    cs = slice(c * 64, (c + 1) * 64)
                    P.MM(psC[0:64, cs], hsel[:, h, :], r4['cl'][:, cs], True, True, ['hsel', 'cl'], ['psC'])
                P.TT(tmpL[:], psC[0:64, :].rearrange("p (a b) -> p a b", b=64),
                     tsm[:, :, 4 + h:5 + h].to_broadcast([64, 8, 64]), ALU.subtract, ['psC', 'tsm'], ['tmpL'])
                P.TS(tmpL[:], tmpL[:], 0.0, None, ALU.min, None, ['tmpL'], ['tmpL'])
                P.ACT(tmpL[:], tmpL[:], AF.Exp, ['tmpL'], ['tmpL'])
                P.TT(ArkT[:, h, :, :], tmpL[:], scT[:], ALU.mult, ['tmpL', 'scT'], ['ArkT'])
            for c in range(8):
                gi = blk * 8 + c
                Sc, Sn = STs[gi % 2], STs[(gi + 1) % 2]
                Scn, Snn = 'ST%d' % (gi % 2), 'ST%d' % ((gi + 1) % 2)
                cs = slice(c * 64, (c + 1) * 64)
                for h in range(4):
                    P.MM(psYi[0:64, h * 64:(h + 1) * 64], ArkT[:, h, c, :], xdt[:, c, h * 64:(h + 1) * 64], True, True,
                         ['ArkT', 'xdt'], ['psYi'])
                P.MM(psYo[0:64, 0:256], cv[:, 3, cs], Sc[:], True, True, ['cv', Scn], ['psYo'])
                P.MM(psN[:, 0:256], tok[:, c, 256:384], xdtd[:, c, :], True, True, ['tok', 'xdtd'], ['psN'])
                P.TT(tmpY[:].rearrange("p (h v) -> p h v", h=4), psYo[0:64, 0:256].rearrange("p (h v) -> p h v", h=4),
                     tsm[:, c, 8:12].unsqueeze(2).to_broadcast([64, 4, 64]), ALU.mult, ['psYo', 'tsm'], ['tmpY'])
                P.TT(Yb[:, c, :], tmpY[:], psYi[0:64, 0:256], ALU.add, ['tmpY', 'psYi'], ['Yb'])
                P.TT(Stmp[:].rearrange("p (h v) -> p h v", h=4), Sc[:].rearrange("p (h v) -> p h v", h=4),
                     gamB[:, c, :].unsqueeze(2).to_broadcast([128, 4, 64]), ALU.mult, [Scn, 'gamB'], ['Stmp'])
                P.TT(Sn[:], Stmp[:], psN[:, 0:256], ALU.add, ['Stmp', 'psN'], [Snn])
            P.TT(Yt[:].rearrange("p c (h v) -> p c h v", h=4), xs4,
                 dv[:].unsqueeze(1).unsqueeze(3).to_broadcast([64, 8, 4, 64]), ALU.mult, ['tok', 'dv'], ['Yt'])
            P.TT(Yb[:], Yb[:], Yt[:], ALU.add, ['Yb', 'Yt'], ['Yb'])
            P.TT(Yt[:], Yb[:], tok[:, :, 384:640], ALU.mult, ['Yb', 'tok'], ['Yt'])
            P.ST(y[blk * 512:(blk + 1) * 512, :].rearrange("(c t) f -> t c f", t=64), Yt[:], ['Yt'])
        P.emit()
    return nc


MB_DI = 2048


def mamba_inputs(hT, core, inp):
    j = 0
    g = core // 2
    w = inp['mb_w_in'][j]
    f = np.float32
    d = {}
    d['xT'] = hT
    zc = core * 256
    d['wz'] = np.ascontiguousarray(np.stack([kl(w[:, zc + i * 128: zc + (i + 1) * 128]) for i in range(2)], 1))
    x0 = MB_DI + core * 256
    b0 = MB_DI + MB_DI + g * 128
    c0 = MB_DI + MB_DI + 512 + g * 128
    cols = [slice(x0, x0 + 128), slice(x0 + 128, x0 + 256), slice(b0, b0 + 128), slice(c0, c0 + 128)]
    d['wx'] = np.ascontiguousarray(np.stack([kl(w[:, s]) for s in cols], 1))
    d['wdt'] = kl(w[:, 5120 + core * 4: 5120 + core * 4 + 4])
    cw = np.zeros((128, 4, 5), f)
    for i, s in enumerate(cols):
        s2 = slice(s.start - MB_DI, s.stop - MB_DI)
        cw[:, i, 0:4] = inp['mb_conv_w'][j][:, s2].T
        cw[:, i, 4] = inp['mb_conv_b'][j][s2]
    d['cw'] = cw
    pcol = np.zeros((4, 4), f)
    pcol[:, 0] = inp['mb_dt_bias'][j][core * 4: core * 4 + 4]
    pcol[:, 1] = inp['mb_a_log'][j][core * 4: core * 4 + 4]
    pcol[:, 2] = 1.0
    d['pcol'] = pcol
    d['dvec'] = np.ascontiguousarray(np.broadcast_to(inp['mb_d'][j][core * 4: core * 4 + 4][None, :], (64, 4)))
    c = _consts()
    d['ident'], d['masks'], d['resetmask'] = c['ident'], c['masks'], c['resetmask']
    hs = np.zeros((4, 4, 64), f)
    for h in range(4):
        hs[h, h, :] = 1
    d['hsel'] = hs
    d['eye4'] = np.eye(4, dtype=f)
    d['ones'] = np.ones((128, 128), f)
    return d


_PROGS = {}


def _prog(key, builder):
    if key not in _PROGS:
        _PROGS[key] = builder()
    return _PROGS[key]


def _featmajor(h, pad):
    T = h.shape[0]
    out = np.zeros((128, 8, pad + T), np.float32)
    out[:, :, pad:] = h.T.reshape(8, 128, T).transpose(1, 0, 2)
    return out


def _run(nc, in_maps):
    res = run_bass_kernel_spmd(nc, in_maps, core_ids=list(range(NCORES)))
    return res.results


def kernel(**inp):
    inp = {k: np.asarray(v) for k, v in inp.items()}
    h = np.ascontiguousarray(inp['x'][0], dtype=np.float32)
    nblk = SEQ // 512
    for layer in range(DEPTH):
        kind = ['rwkv', 'mamba', 'gla'][layer % 3]
        j = layer // 3
        ngvec, ssq, eps = None, None, 1e-5
        if kind == 'rwkv':
            hT = _featmajor(h, 1)
            nc = _prog(('rwkv', nblk), lambda: build_rwkv(nblk))
            outs = _run(nc, [rwkv_inputs(hT, j, c, inp) for c in range(NCORES)])
            Y = np.concatenate([o['y'] for o in outs], axis=1)
        elif kind == 'mamba':
            hT = _featmajor(h, 0)
            nc = _prog(('mamba', nblk), lambda: build_mamba(nblk))
            outs = _run(nc, [mamba_inputs(hT, c, inp) for c in range(NCORES)])
            Y = np.concatenate([o['y'] for o in outs], axis=1)
            ngvec = inp['mb_norm_g'][j]
        else:
            hT = _featmajor(h, 0)
            nc = _prog(('gla', nblk), lambda: build_gla(nblk))
            outs = _run(nc, [gla_inputs(hT, c, inp) for c in range(NCORES)])
            Y = np.concatenate([o['y'] for o in outs], axis=1)
            sq = [o['ssq'].T.reshape(SEQ) for o in outs]
            ssq = np.stack([sq[hd * 2 + hf] for hf in range(2) for hd in range(4)], axis=1)
            ngvec = np.tile(inp['gl_norm_g'][j], 4)
        YT = np.ascontiguousarray(Y.T)
        ncp = _prog(('post', kind), lambda: build_post(kind))
        outs = _run(ncp, [post_inputs(kind, layer, c, h, YT, inp, ngvec=ngvec, ssq=ssq, mix_eps=eps) for c in range(NCORES)])
        h = np.concatenate([o['hout'] for o in outs], axis=0)
    return h[None].astype(np.float32)
```
